# Optimizing a Trainium2 kernel written in Bass

```python
import math
import jax
import jax.numpy as jnp
from jax import lax
import numpy as np

D_MODEL = 2048
BATCH = 2
SEQ = 16384
DEPTH = 2

D_ATTN = D_MODEL // 2
D_SSM = D_MODEL // 2

NSA_HEADS = 16
NSA_KV_HEADS = 2
NSA_GROUP = NSA_HEADS // NSA_KV_HEADS
HEAD_DIM = D_ATTN // NSA_HEADS
CMP_BLOCK = 32
CMP_STRIDE = 16
CMP_HIDDEN = 4 * HEAD_DIM
SLC_BLOCK = 64
SLC_TOPN = 16
WINDOW = 512
Q_BLOCK = 128
FORCE_SCORE = 1e4
NEG_INF = -1e30

REL_BUCKETS = 32
REL_MAX_DIST = 2048

SSM_HEAD_DIM = 64
SSM_HEADS = D_SSM // SSM_HEAD_DIM
SSM_GROUPS = 2
SSM_STATE = 128
CONV_WIDTH = 4
CONV_CH = D_SSM + 2 * SSM_GROUPS * SSM_STATE
SSM_CHUNK = 256

PEER_HEADS = 8
PEER_NKEYS = 128
PEER_EXPERTS = PEER_NKEYS * PEER_NKEYS
PEER_KEY_DIM = 256
PEER_TOPK = 16
PEER_TOKEN_BLOCK = 128

N_Q = NSA_HEADS * HEAD_DIM
N_KV = 6 * NSA_KV_HEADS * HEAD_DIM
N_GATE = 3 * NSA_HEADS
N_Z = D_SSM
N_XBC = CONV_CH
N_DT = SSM_HEADS
N_IN = N_Q + N_KV + N_GATE + N_Z + N_XBC + N_DT

RMS_EPS = 1e-6

kernel_name = 'hybrid_nsa_ssd_peer_block'


def rms_norm(x, w):
    xf = x.astype(jnp.float32)
    y = xf * lax.rsqrt(jnp.mean(xf * xf, axis=-1, keepdims=True) + RMS_EPS)
    return (y * w.astype(jnp.float32)).astype(x.dtype)


def masked_softmax(s, mask):
    p = jax.nn.softmax(jnp.where(mask, s.astype(jnp.float32), NEG_INF), axis=-1)
    return jnp.where(mask, p, 0.0)


def rel_bucket(diff):
    dist = jnp.maximum(diff, 0)
    max_exact = REL_BUCKETS // 2
    log_ratio = jnp.log(jnp.maximum(dist, max_exact).astype(jnp.float32) / max_exact) / math.log(REL_MAX_DIST / max_exact)
    large = max_exact + (log_ratio * (REL_BUCKETS - max_exact)).astype(jnp.int32)
    return jnp.where(dist < max_exact, dist, jnp.minimum(large, REL_BUCKETS - 1))


def compress_blocks(k, pe, w1, w2):
    bsz, s = k.shape[0], k.shape[1]
    n_cmp = (s - CMP_BLOCK) // CMP_STRIDE + 1
    idx = np.arange(n_cmp)[:, None] * CMP_STRIDE + np.arange(CMP_BLOCK)[None, :]
    blocks = k[:, idx] + pe[None, None, :, None, :]
    flat = blocks.transpose(0, 1, 3, 2, 4).reshape(bsz, n_cmp, NSA_KV_HEADS, CMP_BLOCK * HEAD_DIM)
    return jax.nn.gelu(flat @ w1) @ w2


def nsa_attention(q, kv, gate_logits, cmp_pe, cmp_w1, cmp_w2, rel_bias):
    bsz, s, _ = q.shape
    f32 = jnp.float32
    q = q.reshape(bsz, s, NSA_KV_HEADS, NSA_GROUP, HEAD_DIM)
    kv = kv.reshape(bsz, s, 6, NSA_KV_HEADS, HEAD_DIM)
    k_cmp_raw, v_cmp_raw, k_slc, v_slc, k_win, v_win = [kv[:, :, i] for i in range(6)]
    gates = jax.nn.sigmoid(gate_logits.astype(f32)).reshape(bsz, s, NSA_KV_HEADS, NSA_GROUP, 3).astype(q.dtype)

    n_cmp = (s - CMP_BLOCK) // CMP_STRIDE + 1
    k_cmp = compress_blocks(k_cmp_raw, cmp_pe[0], cmp_w1[0], cmp_w2[0])
    v_cmp = compress_blocks(v_cmp_raw, cmp_pe[1], cmp_w1[1], cmp_w2[1])
    cmp_end = jnp.arange(n_cmp) * CMP_STRIDE + CMP_BLOCK - 1

    n_slc = s // SLC_BLOCK
    n_sel = min(SLC_TOPN, n_slc)
    k_blocks = k_slc.reshape(bsz, n_slc, SLC_BLOCK, NSA_KV_HEADS, HEAD_DIM).transpose(0, 3, 1, 2, 4)
    v_blocks = v_slc.reshape(bsz, n_slc, SLC_BLOCK, NSA_KV_HEADS, HEAD_DIM).transpose(0, 3, 1, 2, 4)
    j = np.arange(n_slc)
    lo = np.clip((j * SLC_BLOCK - CMP_BLOCK) // CMP_STRIDE + 1, 0, n_cmp)
    hi = np.clip(-((-(j * SLC_BLOCK + SLC_BLOCK)) // CMP_STRIDE), 0, n_cmp)
    blk = jnp.arange(n_slc)

    pad = ((0, 0), (WINDOW, 0), (0, 0), (0, 0))
    k_win_p = jnp.pad(k_win, pad)
    v_win_p = jnp.pad(v_win, pad)

    rel_kg = rel_bias.reshape(REL_BUCKETS, NSA_KV_HEADS, NSA_GROUP)
    b_idx = jnp.arange(bsz)[:, None, None, None]
    h_idx = jnp.arange(NSA_KV_HEADS)[None, :, None, None]
    kv_idx = jnp.arange(NSA_KV_HEADS)[None, :, None, None, None, None]
    g_idx = jnp.arange(NSA_GROUP)[None, None, :, None, None, None]
    scale = HEAD_DIM ** -0.5

    def head_bias(diff):
        return rel_kg[rel_bucket(diff)].transpose(2, 3, 0, 1)

    def query_block(i):
        qs = i * Q_BLOCK
        t = qs + jnp.arange(Q_BLOCK)
        qb = lax.dynamic_slice_in_dim(q, qs, Q_BLOCK, axis=1)
        gb = lax.dynamic_slice_in_dim(gates, qs, Q_BLOCK, axis=1)

        s_c = jnp.einsum('bqkgd,bnkd->bkgqn', qb, k_cmp).astype(f32) * scale + head_bias(t[:, None] - cmp_end[None, :])
        p_c = masked_softmax(s_c, cmp_end[None, :] <= t[:, None])
        o_c = jnp.einsum('bkgqn,bnkd->bqkgd', p_c.astype(qb.dtype), v_cmp)

        cs = jnp.pad(jnp.cumsum(p_c.sum(axis=2), axis=-1), ((0, 0), (0, 0), (0, 0), (1, 0)))
        imp = cs[..., hi] - cs[..., lo]
        cur = (t // SLC_BLOCK)[:, None]
        forced = (blk == 0) | (blk == cur) | (blk == cur - 1)
        imp = jnp.where(forced, FORCE_SCORE, jnp.where(blk <= cur, imp, -FORCE_SCORE))
        _, sel = lax.top_k(imp, n_sel)

        k_sel = k_blocks[b_idx, h_idx, sel]
        v_sel = v_blocks[b_idx, h_idx, sel]
        pos_s = sel[..., None] * SLC_BLOCK + jnp.arange(SLC_BLOCK)
        diff_s = t[:, None, None] - pos_s
        bias_s = rel_kg[rel_bucket(diff_s)[:, :, None], kv_idx, g_idx]
        s_s = jnp.einsum('bqkgd,bkqnld->bkgqnl', qb, k_sel).astype(f32) * scale + bias_s
        s_s = s_s.reshape(bsz, NSA_KV_HEADS, NSA_GROUP, Q_BLOCK, n_sel * SLC_BLOCK)
        mask_s = (diff_s >= 0).reshape(bsz, NSA_KV_HEADS, Q_BLOCK, n_sel * SLC_BLOCK)[:, :, None]
        p_s = masked_softmax(s_s, mask_s)
        o_s = jnp.einsum('bkgqm,bkqmd->bqkgd', p_s.astype(qb.dtype),
                         v_sel.reshape(bsz, NSA_KV_HEADS, Q_BLOCK, n_sel * SLC_BLOCK, HEAD_DIM))

        kw = lax.dynamic_slice_in_dim(k_win_p, qs, WINDOW + Q_BLOCK, axis=1)
        vw = lax.dynamic_slice_in_dim(v_win_p, qs, WINDOW + Q_BLOCK, axis=1)
        pos_w = qs - WINDOW + jnp.arange(WINDOW + Q_BLOCK)
        diff_w = t[:, None] - pos_w[None, :]
        mask_w = (diff_w >= 0) & (diff_w < WINDOW) & (pos_w[None, :] >= 0)
        s_w = jnp.einsum('bqkgd,bmkd->bkgqm', qb, kw).astype(f32) * scale + head_bias(diff_w)
        p_w = masked_softmax(s_w, mask_w)
        o_w = jnp.einsum('bkgqm,bmkd->bqkgd', p_w.astype(qb.dtype), vw)

        return gb[..., 0:1] * o_c + gb[..., 1:2] * o_s + gb[..., 2:3] * o_w

    out = lax.map(query_block, jnp.arange(s // Q_BLOCK))
    return out.transpose(1, 0, 2, 3, 4, 5).reshape(bsz, s, NSA_HEADS * HEAD_DIM)


def causal_depthwise_conv(u, w, b):
    ch = u.shape[-1]
    out = lax.conv_general_dilated(u, w[:, None, :].astype(u.dtype), window_strides=(1,),
                                   padding=[(CONV_WIDTH - 1, 0)],
                                   dimension_numbers=('NWC', 'WIO', 'NWC'),
                                   feature_group_count=ch)
    return out + b


def ssd_chunked_scan(x, dt, a, bmat, cmat):
    f32 = jnp.float32
    bsz, s, nh, p = x.shape
    ng, ns = bmat.shape[2], bmat.shape[3]
    nj = nh // ng
    pad = (-s) % SSM_CHUNK
    sp = s + pad
    nc = sp // SSM_CHUNK
    ln = SSM_CHUNK

    def padc(u):
        return jnp.pad(u, [(0, 0), (0, pad)] + [(0, 0)] * (u.ndim - 2))

    def chunks(u):
        return jnp.moveaxis(u.reshape((bsz, nc, ln) + u.shape[2:]), 1, 0)

    xc = chunks(padc(x.astype(f32)).reshape(bsz, sp, ng, nj, p))
    dtc = chunks(padc(dt).reshape(bsz, sp, ng, nj))
    bc = chunks(padc(bmat.astype(f32)))
    cc = chunks(padc(cmat.astype(f32)))
    a_gj = a.reshape(ng, nj)
    causal = jnp.tril(jnp.ones((ln, ln), bool))[None, :, :, None, None]

    def step(state, inp):
        xk, dtk, bk, ck = inp
        acs = jnp.cumsum(dtk * a_gj, axis=1)
        seg = acs[:, :, None] - acs[:, None, :]
        decay = jnp.exp(jnp.where(causal, seg, -jnp.inf))
        cb = jnp.einsum('blgn,bsgn->blsg', ck, bk)
        m = cb[..., None] * decay * dtk[:, None]
        y = jnp.einsum('blsgj,bsgjp->blgjp', m, xk)
        y = y + jnp.einsum('blgn,bgjpn->blgjp', ck, state) * jnp.exp(acs)[..., None]
        last = acs[:, -1]
        w = jnp.exp(last[:, None] - acs) * dtk
        state = state * jnp.exp(last)[..., None, None] + jnp.einsum('bsgj,bsgn,bsgjp->bgjpn', w, bk, xk)
        return state, y

    state0 = jnp.zeros((bsz, ng, nj, p, ns), f32)
    _, ys = lax.scan(step, state0, (xc, dtc, bc, cc))
    ys = jnp.moveaxis(ys, 0, 1).reshape(bsz, sp, nh, p)
    return ys[:, :s]


def mamba2_ssd(z, xbc, dt_raw, conv_w, conv_b, dt_bias, a_log, d_skip, norm_w):
    bsz, s, _ = z.shape
    f32 = jnp.float32
    xbc = jax.nn.silu(causal_depthwise_conv(xbc, conv_w, conv_b))
    xs, bm, cm = jnp.split(xbc, [D_SSM, D_SSM + SSM_GROUPS * SSM_STATE], axis=-1)
    xs = xs.reshape(bsz, s, SSM_HEADS, SSM_HEAD_DIM)
    bm = bm.reshape(bsz, s, SSM_GROUPS, SSM_STATE)
    cm = cm.reshape(bsz, s, SSM_GROUPS, SSM_STATE)
    dt = jax.nn.softplus(dt_raw.astype(f32) + dt_bias.astype(f32))
    a = -jnp.exp(a_log.astype(f32))
    y = ssd_chunked_scan(xs, dt, a, bm, cm)
    y = y + d_skip.astype(f32)[:, None] * xs.astype(f32)
    gw = D_SSM // SSM_GROUPS
    y = y.reshape(bsz, s, SSM_GROUPS, gw) * jax.nn.silu(z.astype(f32)).reshape(bsz, s, SSM_GROUPS, gw)
    y = y * lax.rsqrt(jnp.mean(y * y, axis=-1, keepdims=True) + RMS_EPS)
    return (y.reshape(bsz, s, D_SSM) * norm_w.astype(f32)).astype(z.dtype)


def hybrid_mixer(h, w_in, cmp_pe, cmp_w1, cmp_w2, rel_bias, attn_out_norm,
                 conv_w, conv_b, dt_bias, a_log, d_skip, ssm_norm, w_out):
    proj = h @ w_in
    cuts = [int(v) for v in np.cumsum([N_Q, N_KV, N_GATE, N_Z, N_XBC])]
    q, kv, gate_logits, z, xbc, dt_raw = jnp.split(proj, cuts, axis=-1)
    attn = rms_norm(nsa_attention(q, kv, gate_logits, cmp_pe, cmp_w1, cmp_w2, rel_bias), attn_out_norm)
    ssm = mamba2_ssd(z, xbc, dt_raw, conv_w, conv_b, dt_bias, a_log, d_skip, ssm_norm)
    return jnp.concatenate([attn, ssm], axis=-1) @ w_out


def peer_ffn(h, wq, subkeys, u_tab, v_tab):
    bsz, s, d = h.shape
    f32 = jnp.float32
    q = (h @ wq).reshape(bsz, s, PEER_HEADS, 2, PEER_KEY_DIM // 2)
    s1 = jnp.einsum('bshd,kd->bshk', q[:, :, :, 0], subkeys[0]).astype(f32)
    s2 = jnp.einsum('bshd,kd->bshk', q[:, :, :, 1], subkeys[1]).astype(f32)
    v1, i1 = lax.top_k(s1, PEER_TOPK)
    v2, i2 = lax.top_k(s2, PEER_TOPK)
    n_cand = PEER_TOPK * PEER_TOPK
    cand = (v1[..., :, None] + v2[..., None, :]).reshape(bsz, s, PEER_HEADS, n_cand)
    cidx = (i1[..., :, None] * PEER_NKEYS + i2[..., None, :]).reshape(bsz, s, PEER_HEADS, n_cand)
    best, pos = lax.top_k(cand, PEER_TOPK)
    eidx = jnp.take_along_axis(cidx, pos, axis=-1)
    gate = jax.nn.softmax(best, axis=-1).astype(h.dtype)
    n_blk = bsz * s // PEER_TOKEN_BLOCK
    hb = h.reshape(n_blk, PEER_TOKEN_BLOCK, d)
    eb = eidx.reshape(n_blk, PEER_TOKEN_BLOCK, PEER_HEADS * PEER_TOPK)
    gb = gate.reshape(n_blk, PEER_TOKEN_BLOCK, PEER_HEADS * PEER_TOPK)

    def expert_block(args):
        hx, ex, gx = args
        act = jax.nn.gelu(jnp.einsum('td,tkd->tk', hx, u_tab[ex]))
        return jnp.einsum('tk,tkd->td', act * gx, v_tab[ex])

    return lax.map(expert_block, (hb, eb, gb)).reshape(bsz, s, d)


def setup_inputs(seed: int = 0) -> dict:
    key = jax.random.key(seed)
    ks = jax.random.split(key, 24)
    f32 = jnp.float32

    def nrm(k, shape, sd):
        return jax.random.normal(k, shape, f32) * sd

    dt0 = jnp.exp(jax.random.uniform(ks[14], (DEPTH, SSM_HEADS), f32) * (math.log(0.1) - math.log(0.001)) + math.log(0.001))
    return {
        'x': nrm(ks[0], (BATCH, SEQ, D_MODEL), 1.0),
        'c': nrm(ks[1], (BATCH, D_MODEL), 1.0),
        'ada_w': nrm(ks[2], (DEPTH, D_MODEL, 6 * D_MODEL), 0.5 * D_MODEL ** -0.5),
        'ada_b': nrm(ks[3], (DEPTH, 6 * D_MODEL), 0.01),
        'norm_mix': 1.0 + nrm(ks[4], (DEPTH, D_MODEL), 0.05),
        'norm_ffn': 1.0 + nrm(ks[5], (DEPTH, D_MODEL), 0.05),
        'w_in': nrm(ks[6], (DEPTH, D_MODEL, N_IN), D_MODEL ** -0.5),
        'cmp_pe': nrm(ks[7], (DEPTH, 2, CMP_BLOCK, HEAD_DIM), 0.1),
        'cmp_w1': nrm(ks[8], (DEPTH, 2, CMP_BLOCK * HEAD_DIM, CMP_HIDDEN), (CMP_BLOCK * HEAD_DIM) ** -0.5),
        'cmp_w2': nrm(ks[9], (DEPTH, 2, CMP_HIDDEN, HEAD_DIM), CMP_HIDDEN ** -0.5),
        'rel_bias': nrm(ks[10], (REL_BUCKETS, NSA_HEADS), 0.5),
        'attn_out_norm': 1.0 + nrm(ks[11], (DEPTH, D_ATTN), 0.05),
        'conv_w': nrm(ks[12], (DEPTH, CONV_WIDTH, CONV_CH), CONV_WIDTH ** -0.5),
        'conv_b': nrm(ks[13], (DEPTH, CONV_CH), 0.01),
        'dt_bias': dt0 + jnp.log(-jnp.expm1(-dt0)),
        'a_log': jnp.log(jax.random.uniform(ks[15], (DEPTH, SSM_HEADS), f32, 1.0, 16.0)),
        'd_skip': 1.0 + nrm(ks[16], (DEPTH, SSM_HEADS), 0.1),
        'ssm_norm': 1.0 + nrm(ks[17], (DEPTH, D_SSM), 0.05),
        'w_out': nrm(ks[18], (DEPTH, D_MODEL, D_MODEL), D_MODEL ** -0.5),
        'peer_wq': nrm(ks[19], (DEPTH, D_MODEL, PEER_HEADS * PEER_KEY_DIM), D_MODEL ** -0.5),
        'peer_subkeys': nrm(ks[20], (DEPTH, 2, PEER_NKEYS, PEER_KEY_DIM // 2), (PEER_KEY_DIM // 2) ** -0.5),
        'peer_u': nrm(ks[21], (DEPTH, PEER_EXPERTS, D_MODEL), D_MODEL ** -0.5),
        'peer_v': nrm(ks[22], (DEPTH, PEER_EXPERTS, D_MODEL), PEER_HEADS ** -0.5),
        'norm_final': 1.0 + nrm(ks[23], (D_MODEL,), 0.05),
    }


def reference(x, c, ada_w, ada_b, norm_mix, norm_ffn, w_in, cmp_pe, cmp_w1, cmp_w2, rel_bias,
              attn_out_norm, conv_w, conv_b, dt_bias, a_log, d_skip, ssm_norm, w_out,
              peer_wq, peer_subkeys, peer_u, peer_v, norm_final):
    cond = jax.nn.silu(c)
    for l in range(DEPTH):
        mod = (cond @ ada_w[l] + ada_b[l])[:, None, :]
        sh1, sc1, g1, sh2, sc2, g2 = jnp.split(mod, 6, axis=-1)
        h = rms_norm(x, norm_mix[l]) * (1.0 + sc1) + sh1
        x = x + g1 * hybrid_mixer(h, w_in[l], cmp_pe[l], cmp_w1[l], cmp_w2[l], rel_bias, attn_out_norm[l],
                                  conv_w[l], conv_b[l], dt_bias[l], a_log[l], d_skip[l], ssm_norm[l], w_out[l])
        h = rms_norm(x, norm_ffn[l]) * (1.0 + sc2) + sh2
        x = x + g2 * peer_ffn(h, peer_wq[l], peer_subkeys[l], peer_u[l], peer_v[l])
    return rms_norm(x, norm_final)
```

```python
import math
import numpy as np
from contextlib import ExitStack
import concourse.bass as bass
import concourse.mybir as mybir
from concourse.bass_utils import run_bass_kernel_spmd

F32 = mybir.dt.float32
BF16 = mybir.dt.bfloat16
ALU = mybir.AluOpType
AF = mybir.ActivationFunctionType
AX = mybir.AxisListType
NDS = 6


def _key(k):
    if isinstance(k, (str, tuple)):
        return k
    if hasattr(k, 'tensor'):
        return k.tensor.name
    return k.name


class Prog:
    ENG = ['pe', 'act', 'dve', 'pool', 'sp']

    def __init__(self, nc):
        self.nc = nc
        self.es = ExitStack()
        self.ops = {e: [] for e in self.ENG}
        self.cnt = {e: 0 for e in self.ENG}
        self.sem = {}
        for e in ['pe', 'act', 'dve', 'pool']:
            self.sem['c_' + e] = self.es.enter_context(nc.semaphore('c_' + e))
        self.waited = {e: {} for e in self.ENG}
        self.lastw = {}
        self.readers = {}
        self.dsem = {}
        self.dptr = {}
        for q in ['sp', 'pool', 'act']:
            self.dsem[q] = []
            for i in range(NDS):
                n = 'd_%s%d' % (q, i)
                self.sem[n] = self.es.enter_context(nc.semaphore(n))
                self.dsem[q].append([n, 0])
            self.dptr[q] = 0
        self.n_ops = 0

    def sb(self, name, shape, dt):
        return self.es.enter_context(self.nc.sbuf_tensor(name, list(shape), dt))

    def ps(self, name, shape, dt):
        return self.es.enter_context(self.nc.psum_tensor(name, list(shape), dt))

    def _need(self, eng, deps):
        w = self.waited[eng]
        for (s, v) in deps:
            if w.get(s, 0) < v:
                self.ops[eng].append(('wait', s, v))
                w[s] = v

    def _deps(self, reads, writes):
        deps = []
        for k in reads:
            t = self.lastw.get(k)
            if t is not None:
                deps.append(t)
        for k in writes:
            t = self.lastw.get(k)
            if t is not None:
                deps.append(t)
            r = self.readers.get(k)
            if r:
                deps.extend(r.items())
        return deps

    def _commit(self, reads, writes, tok):
        for k in writes:
            self.lastw[k] = tok
            self.readers[k] = {}
        for k in reads:
            if k in writes:
                continue
            r = self.readers.setdefault(k, {})
            if r.get(tok[0], 0) < tok[1]:
                r[tok[0]] = tok[1]

    def op(self, eng, fn, reads=(), writes=()):
        reads = [_key(r) for r in reads]
        writes = [_key(r) for r in writes]
        deps = self._deps(reads, writes)
        if eng == 'pe':
            deps = [d for d in deps if d[0] != 'c_pe']
        self._need(eng, deps)
        self.cnt[eng] += 1
        tok = ('c_' + eng, self.cnt[eng])
        self.ops[eng].append(('op', fn, 'c_' + eng))
        self._commit(reads, writes, tok)
        self.n_ops += 1

    def dma(self, q, out, in_, reads=None, writes=None):
        reads = [_key(r) for r in (reads if reads is not None else [in_])]
        writes = [_key(r) for r in (writes if writes is not None else [out])]
        deps = self._deps(reads, writes)
        slot = self.dsem[q][self.dptr[q]]
        self.dptr[q] = (self.dptr[q] + 1) % NDS
        if slot[1] > 0:
            deps.append((slot[0], slot[1]))
        self._need(q, deps)
        slot[1] += 16
        tok = (slot[0], slot[1])
        self.ops[q].append(('dma', out, in_, slot[0]))
        self._commit(reads, writes, tok)
        self.n_ops += 1

    def barrier(self):
        deps = [('c_' + e, self.cnt[e]) for e in ['pe', 'act', 'dve', 'pool'] if self.cnt[e] > 0]
        for q in self.dsem:
            for s, v in self.dsem[q]:
                if v > 0:
                    deps.append((s, v))
        for e in self.ENG:
            self._need(e, deps)

    def finish(self):
        deps = []
        for q in self.dsem:
            for s, v in self.dsem[q]:
                if v > 0:
                    deps.append((s, v))
        self._need('sp', deps)

    def _replay(self, e, eng):
        sem = self.sem
        for o in self.ops[e]:
            if o[0] == 'wait':
                eng.wait_ge(sem[o[1]], o[2])
            elif o[0] == 'op':
                o[1](eng).then_inc(sem[o[2]], 1)
            else:
                eng.dma_start(out=o[1], in_=o[2]).then_inc(sem[o[3]], 16)

    def emit(self):
        self.finish()
        nc = self.nc
        with nc.Block() as block:
            @block.tensor
            def _(e):
                self._replay('pe', e)

            @block.scalar
            def _(e):
                self._replay('act', e)

            @block.vector
            def _(e):
                self._replay('dve', e)

            @block.gpsimd
            def _(e):
                self._replay('pool', e)

            @block.sync
            def _(e):
                self._replay('sp', e)
        self.es.close()

    def mm(self, out, lhsT, rhs, start=True, stop=True, extra_r=(), wkey=None):
        self.op('pe', lambda e: e.matmul(out, lhsT, rhs, start=start, stop=stop),
                reads=[lhsT, rhs] + list(extra_r), writes=[wkey if wkey is not None else out])

    def tr(self, out, in_, ident):
        self.op('pe', lambda e: e.transpose(out, in_, ident), reads=[in_, ident], writes=[out])

    def actv(self, out, in_, func, bias=None, scale=None, accum_out=None, extra_r=()):
        kw = {}
        r = [in_] + list(extra_r)
        w = [out]
        if bias is not None:
            kw['bias'] = bias
            if not isinstance(bias, (int, float)):
                r.append(bias)
        if scale is not None:
            kw['scale'] = scale
            if not isinstance(scale, (int, float)):
                r.append(scale)
        if accum_out is not None:
            kw['accum_out'] = accum_out
            w.append(accum_out)
        self.op('act', lambda e: e.activation(out, in_, func, **kw), reads=r, writes=w)

    def tt(self, eng, out, in0, in1, op):
        self.op(eng, lambda e: e.tensor_tensor(out, in0, in1, op), reads=[in0, in1], writes=[out])

    def ts(self, eng, out, in0, s1, s2, op0, op1=None, accum_out=None):
        r = [in0]
        for s in (s1, s2):
            if s is not None and not isinstance(s, (int, float)):
                r.append(s)
        w = [out]
        kw = {}
        if op1 is not None:
            kw['op1'] = op1
        if accum_out is not None:
            kw['accum_out'] = accum_out
            w.append(accum_out)
        self.op(eng, lambda e: e.tensor_scalar(out, in0, s1, s2, op0, **kw), reads=r, writes=w)

    def stt(self, eng, out, in0, scalar, in1, op0, op1):
        r = [in0, in1]
        if not isinstance(scalar, (int, float)):
            r.append(scalar)
        self.op(eng, lambda e: e.scalar_tensor_tensor(out, in0, scalar, in1, op0, op1), reads=r, writes=[out])

    def cp(self, eng, out, in_):
        if eng == 'act':
            self.op('act', lambda e: e.copy(out, in_), reads=[in_], writes=[out])
        else:
            self.op(eng, lambda e: e.tensor_copy(out, in_), reads=[in_], writes=[out])

    def memset(self, eng, out, v):
        self.op(eng, lambda e: e.memset(out, v), reads=[], writes=[out])


def run(nc, in_maps, trace=False):
    res = run_bass_kernel_spmd(nc, in_maps, core_ids=list(range(len(in_maps))), trace=trace)
    return res


NTOK = 4096
D = 2048
NIN = 4416


def build_L1(ntok=NTOK):
    nc = bass.Bass("TRN2", target_bir_lowering=False)
    x = nc.dram_tensor("x", [ntok, D], F32, kind="ExternalInput").ap()
    cT = nc.dram_tensor("cT", [128, 16], F32, kind="ExternalInput").ap()
    ada_w = nc.dram_tensor("ada_w", [D, 4096], F32, kind="ExternalInput").ap()
    ada_b = nc.dram_tensor("ada_b", [1, 4096], F32, kind="ExternalInput").ap()
    nw = nc.dram_tensor("nw", [1, D], F32, kind="ExternalInput").ap()
    w_in = nc.dram_tensor("w_in", [D, NIN], F32, kind="ExternalInput").ap()
    ident = nc.dram_tensor("ident", [128, 128], F32, kind="ExternalInput").ap()
    proj = nc.dram_tensor("proj", [ntok, NIN], F32, kind="ExternalOutput").ap()
    p = Prog(nc)
    idb = p.sb("idb", [128, 128], BF16)
    p.dma('pool', idb[:], ident)
    cs = p.sb("cs", [128, 16], F32)
    p.dma('sp', cs[:], cT)
    sg = p.sb("sg", [128, 16], F32)
    p.actv(sg[:], cs[:], AF.Sigmoid)
    p.tt('dve', cs[:], cs[:], sg[:], ALU.mult)
    crep = p.sb("crep", [128, 16, 128], F32)
    p.cp('dve', crep[:], cs[:].unsqueeze(2).to_broadcast([128, 16, 128]))
    modt = p.sb("modt", [128, 4096], F32)
    A = p.sb("A", [128, D], F32)
    pm = [p.ps("pm%d" % i, [128, 512], F32) for i in range(4)]
    ptr = [p.ps("ptr%d" % i, [128, 1024], BF16) for i in range(2)]
    with ExitStack() as es2:
        awb = [es2.enter_context(nc.sbuf_tensor("awb%d" % i, [128, 16, 512], F32)) for i in range(2)]
        adb = es2.enter_context(nc.sbuf_tensor("adb", [128, 4096], F32))
        nwb = es2.enter_context(nc.sbuf_tensor("nwb", [128, D], F32))
        p.dma('sp', adb[:], ada_b.partition_broadcast(128))
        p.dma('sp', nwb[:], nw.partition_broadcast(128))
        awv = ada_w.rearrange("(kc p) n -> p kc n", p=128)
        for cc in range(8):
            b = awb[cc % 2]
            p.dma('sp', b[:], awv[:, :, cc * 512:(cc + 1) * 512])
            for kc in range(16):
                p.mm(pm[cc % 2][:], crep[:, kc, :], b[:, kc, :], start=(kc == 0), stop=(kc == 15))
            p.tt('dve', modt[:, cc * 512:(cc + 1) * 512], pm[cc % 2][:], adb[:, cc * 512:(cc + 1) * 512], ALU.add)
        p.stt('dve', A[:], modt[:, D:2 * D], 1.0, nwb[:], ALU.add, ALU.mult)
    p.barrier()
    sh1 = modt[:, 0:D]
    xt = [p.sb("xt%d" % i, [128, D], F32) for i in range(2)]
    junk = p.sb("junk", [128, D], F32)
    hb = [p.sb("hb%d" % i, [128, D], BF16) for i in range(2)]
    hT = [p.sb("hT%d" % i, [128, 16, 128], BF16) for i in range(2)]
    ss = [p.sb("ss%d" % i, [128, 1], F32) for i in range(2)]
    HC = NIN // 2
    mhalf = p.sb("mhalf", [128, 1], F32)
    p.memset("dve", mhalf[:], -0.5)
    wb = p.sb("wb", [128, 16, HC], BF16)
    ot = [p.sb("ot%d" % i, [128, HC], F32) for i in range(2)]
    wv = w_in.rearrange("(kc p) n -> p kc n", p=128)
    nt = ntok // 128
    it = 0
    for half in range(2):
        c0 = half * HC
        for kc in range(16):
            p.dma('pool', wb[:, kc, :], wv[:, kc, c0:c0 + HC])
        for t in range(nt):
            i2 = it % 2
            it += 1
            p.dma('sp', xt[i2][:], x[t * 128:(t + 1) * 128, :])
            p.actv(junk[:], xt[i2][:], AF.Square, accum_out=ss[i2][:])
            p.ts('dve', ss[i2][:], ss[i2][:], 1.0 / D, 1e-6, ALU.mult, ALU.add)
            p.tt('pool', ss[i2][:], ss[i2][:], mhalf[:], ALU.pow)
            p.stt('dve', xt[i2][:], xt[i2][:], ss[i2][:, 0:1], A[:], ALU.mult, ALU.mult)
            p.tt('dve', hb[i2][:], xt[i2][:], sh1, ALU.add)
            for g in range(2):
                for j in range(8):
                    kc = g * 8 + j
                    p.tr(ptr[g][:, j * 128:(j + 1) * 128], hb[i2][:, kc * 128:(kc + 1) * 128], idb[:])
                if g == 0:
                    p.cp('act', hT[i2][:, 0:8, :], ptr[g][:].rearrange("p (a b) -> p a b", b=128))
                else:
                    p.cp('dve', hT[i2][:, 8:16, :], ptr[g][:].rearrange("p (a b) -> p a b", b=128))
            nch = (HC + 511) // 512
            for j in range(nch):
                w = min(512, HC - j * 512)
                pb = pm[j % 4]
                for kc in range(16):
                    p.mm(pb[:, 0:w], hT[i2][:, kc, :], wb[:, kc, j * 512:j * 512 + w], start=(kc == 0), stop=(kc == 15))
                if j % 2 == 0:
                    p.cp('act', ot[i2][:, j * 512:j * 512 + w], pb[:, 0:w])
                else:
                    p.cp('dve', ot[i2][:, j * 512:j * 512 + w], pb[:, 0:w])
            p.dma('sp', proj[t * 128:(t + 1) * 128, c0:c0 + HC], ot[i2][:])
    p.emit()
    return nc


def build_L2a(S=16384):
    NS = S // 128
    NCH = S // 256
    nc = bass.Bass("TRN2", target_bir_lowering=False)
    dt_ = lambda n, s: nc.dram_tensor(n, s, F32, kind="ExternalInput").ap()
    xT = dt_("xT", [256, S + 3])
    bT = dt_("bT", [128, S + 3])
    cTm = dt_("cTm", [128, S + 3])
    dtr = dt_("dtr", [128, NS, 4])
    cw = dt_("cw", [128, 4, 4])
    cb = dt_("cb", [128, 4])
    hp = dt_("hp", [1, 12])
    tri = dt_("tri", [128, 256])
    msk = dt_("msk", [128, 256])
    ident = dt_("ident", [128, 128])
    y = nc.dram_tensor("y", [S, 256], F32, kind="ExternalOutput").ap()
    p = Prog(nc)
    idf = p.sb("idf", [128, 128], F32)
    p.dma('sp', idf[:], ident)
    trit = p.sb("trit", [128, 256], F32)
    p.dma('sp', trit[:], tri)
    mskt = p.sb("mskt", [128, 256], F32)
    p.dma('sp', mskt[:], msk)
    ones = p.sb("ones", [128, 128], F32)
    p.memset('dve', ones[:], 1.0)
    cwt = p.sb("cwt", [128, 4, 4], F32)
    p.dma('sp', cwt[:], cw)
    cbt = p.sb("cbt", [128, 4], F32)
    p.dma('sp', cbt[:], cb)
    hpt = p.sb("hpt", [128, 12], F32)
    p.dma('sp', hpt[:], hp.partition_broadcast(128))
    dta = p.sb("dta", [128, NS, 4], F32)
    dtA = p.sb("dtA", [128, NS, 4], F32)
    p.dma('sp', dta[:], dtr)
    p.tt('dve', dta[:], dta[:], hpt[:, 0:4].unsqueeze(1).to_broadcast([128, NS, 4]), ALU.add)
    p.actv(dta[:], dta[:], AF.Exp)
    p.actv(dta[:], dta[:], AF.Ln, bias=1.0, scale=1.0)
    am = p.sb("am", [128, 4], F32)
    p.actv(am[:], hpt[:, 4:8], AF.Exp)
    p.ts('dve', am[:], am[:], -1.0, None, ALU.mult)
    p.tt('dve', dtA[:], dta[:], am[:].unsqueeze(1).to_broadcast([128, NS, 4]), ALU.mult)
    Drow = p.sb("Drow", [128, 4, 64], F32)
    p.cp('dve', Drow[:], hpt[:, 8:12].unsqueeze(2).to_broadcast([128, 4, 64]))
    st = p.sb("st", [128, 4, 64], F32)
    stb = p.sb("stb", [128, 256], BF16)
    p.memset('dve', st[:], 0.0)
    p.memset('dve', stb[:], 0.0)
    raw = p.sb("raw", [128, 4, 259], F32)
    conv = p.sb("conv", [128, 4, 256], F32)
    sig = p.sb("sig", [128, 4, 256], F32)
    actf = p.sb("actf", [128, 3, 256], F32)
    bcT = p.sb("bcT", [128, 2, 256], BF16)
    xtm = p.sb("xtm", [128, 2, 256], F32)
    xtb = p.sb("xtb", [128, 2, 256], BF16)
    Btm = p.sb("Btm", [128, 2, 128], BF16)
    nacs = p.sb("nacs", [128, 2, 4], F32)
    eacs = p.sb("eacs", [128, 2, 4], F32)
    cbs = p.sb("cbs", [128, 2, 256], F32)
    drep = [p.sb("drep%d" % i, [128, 2, 128], F32) for i in range(2)]
    tmp = [p.sb("tmp%d" % i, [128, 384], F32) for i in range(2)]
    dec = [p.sb("dec%d" % i, [128, 384], F32) for i in range(2)]
    MT = [p.sb("MT%d" % i, [128, 384], BF16) for i in range(2)]
    lastrep = p.sb("lastrep", [128, 4], F32)
    elast = p.sb("elast", [128, 4], F32)
    wts = p.sb("wts", [128, 2, 4], F32)
    xw = p.sb("xw", [128, 2, 256], BF16)
    yo = p.sb("yo", [128, 2, 256], F32)
    yd = p.sb("yd", [128, 2, 256], F32)
    ptx = p.ps("ptx", [128, 512], F32)
    pmisc = p.ps("pmisc", [128, 512], F32)
    pcb = p.ps("pcb", [128, 512], F32)
    par = [p.ps("par%d" % i, [128, 512], F32) for i in range(2)]
    pyi = p.ps("pyi", [128, 512], F32)
    pyo = p.ps("pyo", [128, 512], F32)
    pst = p.ps("pst", [128, 512], F32)
    yv = y.rearrange("(n k p) c -> n p k c", p=128, k=2)
    for c in range(NCH):
        T0 = c * 256
        s0, s1 = 2 * c, 2 * c + 1
        p.dma('sp', raw[:, 0:2, :], xT[:, T0:T0 + 259].rearrange("(a p) t -> p a t", p=128))
        p.dma('sp', raw[:, 2, :], bT[:, T0:T0 + 259])
        p.dma('sp', raw[:, 3, :], cTm[:, T0:T0 + 259])
        for i in range(4):
            p.ts('dve', conv[:, i, :], raw[:, i, 0:256], cwt[:, i, 0:1], cbt[:, i:i + 1], ALU.mult, ALU.add)
            for k in range(1, 4):
                p.stt('dve', conv[:, i, :], raw[:, i, k:k + 256], cwt[:, i, k:k + 1], conv[:, i, :], ALU.mult, ALU.add)
        p.actv(sig[:], conv[:], AF.Sigmoid)
        p.tt('dve', actf[:], conv[:, 0:3, :], sig[:, 0:3, :], ALU.mult)
        p.tt('dve', bcT[:], conv[:, 2:4, :], sig[:, 2:4, :], ALU.mult)
        for a in range(2):
            for k in range(2):
                p.tr(ptx[:, k * 256 + a * 128:k * 256 + (a + 1) * 128], actf[:, a, k * 128:(k + 1) * 128], idf[:])
        for k in range(2):
            p.tr(pmisc[:, k * 128:(k + 1) * 128], actf[:, 2, k * 128:(k + 1) * 128], idf[:])
        p.cp('act', xtm[:], ptx[:].rearrange("p (k c) -> p k c", k=2))
        p.cp('dve', xtb[:], xtm[:])
        p.cp('act', Btm[:], pmisc[:, 0:256].rearrange("p (k c) -> p k c", k=2))
        p.mm(pmisc[:, 256:260], trit[:, 0:128], dtA[:, s0, :], start=True, stop=True)
        p.mm(pmisc[:, 260:264], ones[:], dtA[:, s0, :], start=True, stop=False)
        p.mm(pmisc[:, 260:264], trit[:, 0:128], dtA[:, s1, :], start=False, stop=True)
        acs_ps = pmisc[:, 256:264].rearrange("p (k h) -> p k h", k=2)
        p.ts('dve', nacs[:], acs_ps, -1.0, None, ALU.mult)
        p.actv(eacs[:], acs_ps, AF.Exp)
        for k in range(2):
            p.mm(pcb[:, k * 256:(k + 1) * 256], bcT[:, 0, k * 128:(k + 1) * 128], bcT[:, 1, :], start=True, stop=True)
        p.cp('act', cbs[:], pcb[:].rearrange("p (k c) -> p k c", k=2))
        for hh in range(4):
            i2 = hh % 2
            hc = slice(hh * 64, (hh + 1) * 64)
            for k in range(2):
                p.ts('dve', drep[i2][:, k, :], ones[:], dtA[:, s0 + k, hh:hh + 1], None, ALU.mult)
            pr = par[i2]
            p.mm(pr[:, 0:128], drep[i2][:, 0, :], trit[:, 0:128], start=True, stop=True)
            p.mm(pr[:, 128:256], drep[i2][:, 0, :], trit[:, 128:256], start=True, stop=False)
            p.mm(pr[:, 128:256], drep[i2][:, 1, :], trit[:, 0:128], start=False, stop=True)
            p.tt('dve', tmp[i2][:, 0:256], pr[:, 0:256], mskt[:], ALU.add)
            p.tt('dve', tmp[i2][:, 256:384], pr[:, 128:256], mskt[:, 0:128], ALU.add)
            p.actv(dec[i2][:, 0:256], tmp[i2][:, 0:256], AF.Exp, bias=nacs[:, 0, hh:hh + 1], scale=1.0)
            p.actv(dec[i2][:, 256:384], tmp[i2][:, 256:384], AF.Exp, bias=nacs[:, 1, hh:hh + 1], scale=1.0)
            p.stt('dve', MT[i2][:, 0:256], dec[i2][:, 0:256], dta[:, s0, hh:hh + 1], cbs[:, 0, :], ALU.mult, ALU.mult)
            p.stt('dve', MT[i2][:, 256:384], dec[i2][:, 256:384], dta[:, s1, hh:hh + 1], cbs[:, 1, 128:256], ALU.mult, ALU.mult)
            p.cp('act', lastrep[:, hh:hh + 1], pr[:, 255:256])
            p.mm(pyi[:, hh * 64:(hh + 1) * 64], MT[i2][:, 0:128], xtb[:, 0, hc], start=True, stop=True)
            p.mm(pyi[:, 256 + hh * 64:256 + (hh + 1) * 64], MT[i2][:, 128:256], xtb[:, 0, hc], start=True, stop=False)
            p.mm(pyi[:, 256 + hh * 64:256 + (hh + 1) * 64], MT[i2][:, 256:384], xtb[:, 1, hc], start=False, stop=True)
            for lt in range(2):
                p.mm(pyo[:, lt * 256 + hh * 64:lt * 256 + (hh + 1) * 64], bcT[:, 1, lt * 128:(lt + 1) * 128], stb[:, hc],
                     start=True, stop=True)
        p.cp('act', yo[:], pyo[:].rearrange("p (k c) -> p k c", k=2))
        yo4 = yo[:].rearrange("p k (h d) -> p k h d", h=4)
        p.tt('dve', yo4, yo4, eacs[:].unsqueeze(3).to_broadcast([128, 2, 4, 64]), ALU.mult)
        p.tt('dve', yo[:], yo[:], pyi[:].rearrange("p (k c) -> p k c", k=2), ALU.add)
        p.tt('dve', yd[:], xtm[:], Drow[:].rearrange("p h d -> p (h d)").unsqueeze(1).to_broadcast([128, 2, 256]), ALU.mult)
        p.tt('dve', yd[:], yd[:], yo[:], ALU.add)
        p.dma('sp', yv[c], yd[:])
        p.actv(elast[:], lastrep[:], AF.Exp)
        for hh in range(4):
            p.actv(wts[:, :, hh], nacs[:, :, hh], AF.Exp, bias=lastrep[:, hh:hh + 1], scale=1.0)
        p.tt('dve', wts[:], wts[:], dta[:, s0:s0 + 2, :], ALU.mult)
        p.tt('dve', xw[:].rearrange("p k (h d) -> p k h d", h=4), xtm[:].rearrange("p k (h d) -> p k h d", h=4),
             wts[:].unsqueeze(3).to_broadcast([128, 2, 4, 64]), ALU.mult)
        p.mm(pst[:, 0:256], Btm[:, 0, :], xw[:, 0, :], start=True, stop=False)
        p.mm(pst[:, 0:256], Btm[:, 1, :], xw[:, 1, :], start=False, stop=True)
        p.tt('dve', st[:], st[:], elast[:].unsqueeze(2).to_broadcast([128, 4, 64]), ALU.mult)
        p.tt('dve', st[:], st[:], pst[:, 0:256].rearrange("p (h d) -> p h d", h=4), ALU.add)
        p.cp('dve', stb[:], st[:].rearrange("p h d -> p (h d)"))
    p.emit()
    return nc


def consts_L2a():
    s = np.arange(128)[:, None]
    l = np.arange(256)[None, :]
    tri = (s <= l).astype(np.float32)
    msk = np.where(s <= l, 0.0, -30000.0).astype(np.float32)
    return {"tri": tri, "msk": msk, "ident": np.eye(128, dtype=np.float32)}


def host_L2a(xbc_x, xbc_b, xbc_c, dt_raw, conv_w_l, conv_b_l, dt_bias_l, a_log_l, d_skip_l, r):
    S = xbc_x.shape[0]
    f = np.float32
    pad = lambda a: np.ascontiguousarray(np.concatenate([np.zeros((a.shape[1], 3), f), a.T], axis=1))
    g = r // 2
    chx = np.arange(r * 256, (r + 1) * 256)
    chb = 1024 + g * 128 + np.arange(128)
    chc = 1024 + 256 + g * 128 + np.arange(128)
    chs = np.stack([chx[:128], chx[128:], chb, chc], 0)
    cw = np.ascontiguousarray(conv_w_l[:, chs].transpose(2, 1, 0))
    cb = np.ascontiguousarray(conv_b_l[chs].T)
    hs = slice(4 * r, 4 * r + 4)
    hp = np.concatenate([dt_bias_l[hs], a_log_l[hs], d_skip_l[hs]])[None].astype(f)
    m = {"xT": pad(xbc_x), "bT": pad(xbc_b), "cTm": pad(xbc_c),
         "dtr": np.ascontiguousarray(dt_raw.reshape(S // 128, 128, 4).transpose(1, 0, 2)),
         "cw": cw, "cb": cb, "hp": hp}
    m.update(consts_L2a())
    return m


OFF = 2200


def table_len(S):
    return ((S + 200 + OFF + 511) // 512) * 512


def rel_bucket_np(diff):
    dist = np.maximum(diff, 0)
    lr = np.log(np.maximum(dist, 16).astype(np.float32) / np.float32(16)) / np.float32(math.log(2048 / 16))
    large = 16 + (lr.astype(np.float32) * np.float32(16)).astype(np.int32)
    return np.where(dist < 16, dist, np.minimum(large, 31))


def build_L2b(S=16384, nqb=None):
    NQB = S // 128
    NCMP = (S - 32) // 16 + 1
    NCT = (NCMP + 127) // 128
    NCP = NCT * 128
    NSB = S // 64
    L = table_len(S)
    myblocks = [2 * ii + 1 for ii in range(NQB // 2)]
    if nqb is not None:
        myblocks = myblocks[:nqb]
    nc = bass.Bass("TRN2", target_bir_lowering=False)
    dt_ = lambda n, s: nc.dram_tensor(n, s, F32, kind="ExternalInput").ap()
    qT = dt_("qT", [64, 8, S // 2])
    kcT = dt_("kcT", [64, S])
    vcT = dt_("vcT", [64, S])
    ksT = dt_("ksT", [64, S])
    vs = dt_("vs", [S, 64])
    kwT = dt_("kwT", [64, S])
    vw = dt_("vw", [S, 64])
    gl = dt_("gl", [S // 2, 24])
    pe = dt_("pe", [64, 2, 32])
    w1 = dt_("w1", [64, 2, 32, 256])
    w2 = dt_("w2", [128, 2, 2, 64])
    relb = dt_("relb", [32, 8])
    oh = dt_("oh", [33, L])
    Amat = dt_("Amat", [128, NCT, NSB])
    Gfix = dt_("Gfix", [128, 2 * NSB])
    wmask = dt_("wmask", [128, 2, 128])
    ind = dt_("ind", [128, 2])
    tabA = nc.dram_tensor("tabA", [8, 128 * (L + 1)], F32, kind="Internal")
    tabB = nc.dram_tensor("tabB", [8, 128 * (L + 16)], F32, kind="Internal")
    o = nc.dram_tensor("o", [len(myblocks) * 128, 512], F32, kind="ExternalOutput").ap()
    p = Prog(nc)
    PS = [p.ps("PS%d" % i, [128, 1024], F32) for i in range(2)]
    PVZ = p.ps("PVZ", [128, 1024], F32)
    PSM = p.ps("PSM", [128, 512], F32)
    PM = p.ps("PM", [128, 512], F32)
    KcT = p.sb("KcT", [64, NCP], BF16)
    Vc = p.sb("Vc", [128, NCT, 64], BF16)
    onesb = p.sb("onesb", [128, 128], BF16)
    p.memset('dve', onesb[:], 1.0)
    with ExitStack() as es0:
        sbt = lambda n, s, d: es0.enter_context(nc.sbuf_tensor(n, s, d))
        rb = sbt("rb", [33, 8], F32)
        p.memset('dve', rb[:], 1.0)
        p.dma('sp', rb[0:32, :], relb)
        rbrep = sbt("rbrep", [33, 8, 128], F32)
        p.cp('dve', rbrep[:], rb[:].unsqueeze(2).to_broadcast([33, 8, 128]))
        ohc = [sbt("ohc%d" % i, [33, 512], F32) for i in range(2)]
        bv = [sbt("bv%d" % i, [128, 512], F32) for i in range(4)]
        k = 0
        for ch in range(L // 512):
            oc_ = ohc[ch % 2]
            p.dma('sp', oc_[:], oh[:, ch * 512:(ch + 1) * 512])
            for g in range(8):
                pq = PS[k % 2]
                b_ = bv[k % 4]
                p.mm(pq[:, 0:512], rbrep[:, g, :], oc_[:], start=True, stop=True)
                if k % 2 == 0:
                    p.cp('act', b_[:], pq[:, 0:512])
                else:
                    p.cp('dve', b_[:], pq[:, 0:512])
                for tab, s_ in ((tabA, 1), (tabB, 16)):
                    dst = bass.AP(tab, g * 128 * (L + s_) + ch * 512, [[L + s_, 128], [1, 512]])
                    p.dma('sp' if s_ == 1 else 'act', dst, b_[:], writes=[tab.name + str(g)])
                k += 1
    p.barrier()

    def bias_src(tab, s_, c):
        return bass.AP(tab, c + OFF, [[L, 128], [128 * (L + s_), 8], [1, 128]])

    with ExitStack() as esA:
        sbt = lambda n, s, d: esA.enter_context(nc.sbuf_tensor(n, s, d))
        rawT = sbt("rawT", [64, S], BF16)
        w1b = sbt("w1b", [64, 32, 256], BF16)
        peb = sbt("peb", [64, 32], BF16)
        w2b = sbt("w2b", [128, 2, 64], BF16)
        GT = sbt("GTc", [128, 2, NCP], BF16)
        pbias = sbt("pbias", [128, 2], F32)
        p.memset('dve', GT[:], 0.0)
        for kv in range(2):
            p.dma('pool', rawT[:], kcT if kv == 0 else vcT)
            p.dma('pool', w1b[:], w1[:, kv, :, :])
            p.dma('pool', peb[:], pe[:, kv, :])
            p.dma('pool', w2b[:], w2[:, kv, :, :])
            for ht in range(2):
                for j in range(32):
                    p.mm(PSM[:, ht:ht + 1], w1b[:, j, ht * 128:(ht + 1) * 128], peb[:, j:j + 1], start=(j == 0), stop=(j == 31))
            p.cp('dve', pbias[:], PSM[:, 0:2])
            for ht in range(2):
                for n0 in range(0, NCMP, 512):
                    nn = min(512, NCMP - n0)
                    pq = PS[(n0 // 512) % 2]
                    for j in range(32):
                        p.mm(pq[:, 0:nn], w1b[:, j, ht * 128:(ht + 1) * 128], rawT[:, 16 * n0 + j:16 * n0 + j + 16 * (nn - 1) + 1:16],
                             start=(j == 0), stop=(j == 31))
                    p.actv(GT[:, ht, n0:n0 + nn], pq[:, 0:nn], AF.Gelu_apprx_tanh, bias=pbias[:, ht:ht + 1], scale=1.0)
            if kv == 0:
                for n0 in range(0, NCP, 512):
                    nn = min(512, NCP - n0)
                    for ht in range(2):
                        p.mm(PM[0:64, 0:nn], w2b[:, ht, :], GT[:, ht, n0:n0 + nn], start=(ht == 0), stop=(ht == 1))
                    p.cp('act', KcT[:, n0:n0 + nn], PM[0:64, 0:nn])
            else:
                for m in range(NCT):
                    for ht in range(2):
                        p.mm(PM[:, 0:64], GT[:, ht, m * 128:(m + 1) * 128], w2b[:, ht, :], start=(ht == 0), stop=(ht == 1))
                    p.cp('act', Vc[:, m, :], PM[:, 0:64])
    p.barrier()
    KsT = p.sb("KsT", [64, S], BF16)
    p.dma('pool', KsT[:], ksT)
    Vs = p.sb("Vs", [128, NQB, 64], BF16)
    vsv = vs.rearrange("(t p) d -> p t d", p=128)
    for t8 in range(0, NQB, 8):
        p.dma('pool', Vs[:, t8:t8 + 8, :], vsv[:, t8:t8 + 8, :])
    Ab = p.sb("Ab", [128, NCT, NSB], BF16)
    p.dma('pool', Ab[:], Amat)
    Gf = p.sb("Gf", [128, 2 * NSB], F32)
    p.dma('sp', Gf[:], Gfix)
    wm = p.sb("wm", [128, 2, 128], F32)
    p.dma('sp', wm[:], wmask)
    indb = p.sb("indb", [128, 2], BF16)
    p.dma('pool', indb[:], ind)
    farb8 = p.sb("farb8", [128, 8], F32)
    p.dma('sp', farb8[:], relb[31:32, :].partition_broadcast(128))
    farB = p.sb("farB", [128, 8, 128], F32)
    p.cp('dve', farB[:], farb8[:].unsqueeze(2).to_broadcast([128, 8, 128]))
    ET = p.sb("ET", [128, NCT, 1024], BF16)
    SF = [p.sb("SF%d" % i, [128, 1024], F32) for i in range(2)]
    PT = [p.sb("PT%d" % i, [128, 1024], BF16) for i in range(2)]
    BT = [p.sb("BT%d" % i, [128, 8, 128], F32) for i in range(3)]
    WT = p.sb("WT", [128, 6, 1024], BF16)
    qb = [p.sb("qb%d" % i, [64, 8, 128], BF16) for i in range(2)]
    kwb = p.sb("kwb", [64, 768], BF16)
    vwb = p.sb("vwb", [128, 6, 64], BF16)
    glt = p.sb("glt", [128, 24], F32)
    gat = p.sb("gat", [128, 8, 3], F32)
    rzs = p.sb("rzs", [128, 1024], F32)
    ocs = p.sb("ocs", [128, 512], F32)
    accO = p.sb("accO", [128, 512], F32)
    accZ = p.sb("accZ", [128, 8], F32)
    zw = p.sb("zw", [128, 8], F32)
    impm = p.sb("impm", [128, NSB], F32)
    wk = p.sb("wk", [128, NSB], F32)
    m8 = p.sb("m8", [128, 16], F32)
    sel = p.sb("sel", [128, NSB], F32)
    res = p.sb("res", [128, 512], F32)
    tmpo = p.sb("tmpo", [128, 512], F32)
    cnt = {'s': 0, 'b': 0}

    def stile(KT_ap, qbuf, tab, s_, c, far, dst_bf, extra=None):
        k = cnt['s']
        cnt['s'] += 1
        ps = PS[k % 2]
        sf = SF[k % 2]
        p.mm(ps[:, 0:512], KT_ap, qbuf[:, 0:4, :].rearrange("p g f -> p (g f)"), start=True, stop=True)
        p.mm(ps[:, 512:1024], KT_ap, qbuf[:, 4:8, :].rearrange("p g f -> p (g f)"), start=True, stop=True)
        if far:
            bt = farB
        else:
            bt = BT[cnt['b'] % 3]
            cnt['b'] += 1
            p.dma('sp', bt[:], bias_src(tab, s_, c), reads=[tab.name + str(g) for g in range(8)])
        p.stt('dve', sf[:], ps[:], 0.125, bt[:].rearrange("p g f -> p (g f)"), ALU.mult, ALU.add)
        if extra is not None:
            sf3 = sf[:].rearrange("p (g f) -> p g f", g=8)
            p.tt('dve', sf3, sf3, extra.unsqueeze(1).to_broadcast([128, 8, 128]), ALU.add)
        p.actv(dst_bf, sf[:], AF.Exp)

    for bi, i in enumerate(myblocks):
        qs = i * 128
        qbuf = qb[bi % 2]
        p.dma('pool', qbuf[:], qT[:, :, bi * 128:(bi + 1) * 128])
        p.dma('sp', glt[:], gl[bi * 128:(bi + 1) * 128, :])
        p.actv(gat[:].rearrange("p g b -> p (g b)"), glt[:], AF.Sigmoid)
        ntc = (8 * i + 7 + 127) // 128
        for m in range(ntc):
            c = 128 * i - 2048 * m - 31
            stile(KcT[:, m * 128:(m + 1) * 128], qbuf, tabB, 16, c, c - 2032 - 128 >= 1528, ET[:, m, :])
        for m in range(ntc):
            for hf in range(2):
                p.mm(PVZ[:, hf * 512:(hf + 1) * 512], onesb[:], ET[:, m, hf * 512:(hf + 1) * 512], start=(m == 0), stop=(m == ntc - 1))
        p.ts('dve', rzs[:], PVZ[:], 1e-30, None, ALU.max)
        p.op('dve', lambda e: e.reciprocal(rzs[:], rzs[:]), reads=[rzs], writes=[rzs])
        for m in range(ntc):
            p.tt('dve', ET[:, m, :], ET[:, m, :], rzs[:], ALU.mult)
        for g in range(8):
            for m in range(ntc):
                p.mm(PM[:, g * 64:(g + 1) * 64], ET[:, m, g * 128:(g + 1) * 128], Vc[:, m, :], start=(m == 0), stop=(m == ntc - 1))
        p.cp('act', ocs[:], PM[:])
        for g in range(8):
            for m in range(ntc):
                p.mm(PSM[:, 0:NSB], ET[:, m, g * 128:(g + 1) * 128], Ab[:, m, :], start=(g == 0 and m == 0), stop=(g == 7 and m == ntc - 1))
        p.tt('dve', impm[:], PSM[:, 0:NSB], Gf[:, NSB - 2 * i:2 * NSB - 2 * i], ALU.add)
        p.ts('dve', impm[:, 0:1], impm[:, 0:1], 1e4, None, ALU.add)
        p.op('dve', lambda e: e.max(m8[:, 0:8], impm[:]), reads=[impm], writes=[m8])
        p.op('dve', lambda e: e.match_replace(wk[:], m8[:, 0:8], impm[:], -1e9), reads=[impm, m8], writes=[wk])
        p.op('dve', lambda e: e.max(m8[:, 8:16], wk[:]), reads=[wk], writes=[m8])
        p.ts('dve', sel[:], impm[:], m8[:, 15:16], None, ALU.is_ge)
        p.memset('dve', accO[:], 0.0)
        p.memset('dve', accZ[:], 0.0)
        for kt in range(i + 1):
            c = qs - kt * 128
            k = cnt['s']
            pt = PT[k % 2]
            stile(KsT[:, kt * 128:(kt + 1) * 128], qbuf, tabA, 1, c, c - 255 >= 1528, pt[:])
            for blk in range(2):
                for g in range(8):
                    p.mm(PVZ[:, blk * 512 + g * 64:blk * 512 + (g + 1) * 64], pt[blk * 64:(blk + 1) * 64, g * 128:(g + 1) * 128],
                         Vs[blk * 64:(blk + 1) * 64, kt, :], start=True, stop=True)
            for g in range(8):
                p.mm(PSM[:, 256 + g * 2:256 + (g + 1) * 2], pt[:, g * 128:(g + 1) * 128], indb[:], start=True, stop=True)
            for blk in range(2):
                sc_ = sel[:, 2 * kt + blk:2 * kt + blk + 1]
                p.stt('dve', accO[:], PVZ[:, blk * 512:(blk + 1) * 512], sc_, accO[:], ALU.mult, ALU.add)
                p.stt('dve', accZ[:], PSM[:, 256 + blk:256 + 16:2], sc_, accZ[:], ALU.mult, ALU.add)
        j0 = max(0, 4 - 2 * (i // 2))
        ks0 = qs - 640 + 128 * j0
        nj = 6 - j0
        p.dma('pool', kwb[:, 0:nj * 128], kwT[:, ks0:qs + 128])
        p.dma('pool', vwb[:, 0:nj, :], vw[ks0:qs + 128, :].rearrange("(j p) d -> p j d", p=128))
        for j in range(j0, 6):
            c = 640 - 128 * j
            stile(kwb[:, (j - j0) * 128:(j - j0 + 1) * 128], qbuf, tabA, 1, c, False, WT[:, j, :], extra=(wm[:, j, :] if j < 2 else None))
        for g in range(8):
            for j in range(j0, 6):
                p.mm(PM[:, g * 64:(g + 1) * 64], WT[:, j, g * 128:(g + 1) * 128], vwb[:, j - j0, :], start=(j == j0), stop=(j == 5))
        for g in range(8):
            for j in range(j0, 6):
                p.mm(PSM[:, 300 + g:301 + g], WT[:, j, g * 128:(g + 1) * 128], onesb[:, 0:1], start=(j == j0), stop=(j == 5))
        p.ts('dve', accZ[:], accZ[:], 1e-30, None, ALU.max)
        p.op('dve', lambda e: e.reciprocal(accZ[:], accZ[:]), reads=[accZ], writes=[accZ])
        p.tt('dve', accZ[:], accZ[:], gat[:, :, 1], ALU.mult)
        p.ts('dve', zw[:], PSM[:, 300:308], 1e-30, None, ALU.max)
        p.op('dve', lambda e: e.reciprocal(zw[:], zw[:]), reads=[zw], writes=[zw])
        p.tt('dve', zw[:], zw[:], gat[:, :, 2], ALU.mult)
        v3 = lambda t: t[:].rearrange("p (g d) -> p g d", g=8)
        bc = lambda a: a.unsqueeze(2).to_broadcast([128, 8, 64])
        p.tt('dve', v3(res), v3(ocs), bc(gat[:, :, 0]), ALU.mult)
        p.tt('dve', v3(tmpo), v3(accO), bc(accZ[:]), ALU.mult)
        p.tt('dve', res[:], res[:], tmpo[:], ALU.add)
        p.tt('dve', v3(tmpo), v3(PM), bc(zw[:]), ALU.mult)
        p.tt('dve', res[:], res[:], tmpo[:], ALU.add)
        p.dma('sp', o[bi * 128:(bi + 1) * 128, :], res[:])
    p.emit()
    return nc


def consts_L2b(S, qhalf):
    L = table_len(S)
    NCMP = (S - 32) // 16 + 1
    NCT = (NCMP + 127) // 128
    NSB = S // 64
    sh = 128 * (1 - qhalf)
    d = np.arange(L) - OFF - sh
    bk = rel_bucket_np(d)
    oh = np.zeros((33, L), np.float32)
    oh[bk, np.arange(L)] = 1.0
    oh[:, d < 0] = 0.0
    oh[32, d < 0] = -30000.0
    n = np.arange(NCT * 128)[:, None]
    j = np.arange(NSB)[None, :]
    lo = np.clip((j * 64 - 32) // 16 + 1, 0, NCMP)
    hi = np.clip(-((-(j * 64 + 64)) // 16), 0, NCMP)
    A = ((n >= lo) & (n < hi) & (n < NCMP)).astype(np.float32)
    Amat = np.ascontiguousarray(A.reshape(NCT, 128, NSB).transpose(1, 0, 2))
    q = np.arange(128)[:, None]
    r = np.arange(2 * NSB)[None, :] - NSB + 2 * (1 - qhalf)
    cq = q // 64
    G = np.where((r == cq) | (r == cq - 1), 1e4, np.where(r > cq, -1e4, 0.0)).astype(np.float32)
    pp = np.arange(128)[:, None]
    ff = np.arange(128)[None, :]
    wmask = np.where(ff >= pp, -30000.0, 0.0).astype(np.float32)
    full = np.full((128, 128), -30000.0, np.float32)
    zero = np.zeros((128, 128), np.float32)
    wm = np.stack([full, wmask], 1) if qhalf == 1 else np.stack([wmask, zero], 1)
    ind = np.zeros((128, 2), np.float32)
    ind[:64, 0] = 1.0
    ind[64:, 1] = 1.0
    return {"oh": oh, "Amat": Amat, "Gfix": G, "wmask": np.ascontiguousarray(wm), "ind": ind}


def host_L2b(q, kv, gate_logits, cmp_pe_l, cmp_w1_l, cmp_w2_l, rel_bias, kvh, qhalf):
    S = q.shape[0]
    f = np.float32
    kv6 = kv.reshape(S, 6, 2, 64)[:, :, kvh, :]
    T = lambda a: np.ascontiguousarray(a.T)
    m = {
        "qT": np.ascontiguousarray(q.reshape(S // 256, 2, 128, 2, 8, 64)[:, qhalf, :, kvh].transpose(3, 2, 0, 1).reshape(64, 8, S // 2)),
        "kcT": T(kv6[:, 0]), "vcT": T(kv6[:, 1]), "ksT": T(kv6[:, 2]), "vs": np.ascontiguousarray(kv6[:, 3]),
        "kwT": T(kv6[:, 4]), "vw": np.ascontiguousarray(kv6[:, 5]),
        "gl": np.ascontiguousarray(gate_logits.reshape(S // 256, 2, 128, 2, 24)[:, qhalf, :, kvh].reshape(S // 2, 24)),
        "pe": np.ascontiguousarray(cmp_pe_l.transpose(2, 0, 1)),
        "w1": np.ascontiguousarray(cmp_w1_l.reshape(2, 32, 64, 256).transpose(2, 0, 1, 3)),
        "w2": np.ascontiguousarray(cmp_w2_l.reshape(2, 2, 128, 64).transpose(2, 0, 1, 3)),
        "relb": np.ascontiguousarray(rel_bias[:, kvh * 8:(kvh + 1) * 8]),
    }
    m.update(consts_L2b(S, qhalf))
    return m


NE = 16384


def rstd_op(p, ss, mhalf, n, eps=1e-6):
    p.ts('dve', ss, ss, 1.0 / n, eps, ALU.mult, ALU.add)
    p.tt('pool', ss, ss, mhalf, ALU.pow)


def build_L3(ntok=4096, last=False, nec=32):
    nc = bass.Bass("TRN2", target_bir_lowering=False)
    dt = lambda n, s: nc.dram_tensor(n, s, F32, kind="ExternalInput").ap()
    x = dt("x", [ntok, D])
    attn = dt("attn", [ntok, 1024])
    yssm = dt("yssm", [ntok, 1024])
    zz = dt("z", [ntok, 1024])
    cT = dt("cT", [128, 16])
    ada_w = dt("ada_w", [D, 8192])
    ada_b = dt("ada_b", [1, 8192])
    nrm = dt("nrm", [1, D])
    nffn = dt("nffn", [1, D])
    nfin = dt("nfin", [1, D])
    w_out = dt("w_out", [D, D])
    wq = dt("wq", [D, D])
    skT = dt("skT", [128, 2, 128])
    uT = dt("uT", [D, NE])
    vv = dt("v", [NE, D])
    ident = dt("ident", [128, 128])
    out = nc.dram_tensor("out", [ntok, D], F32, kind="ExternalOutput").ap()
    p = Prog(nc)
    idb = p.sb("idb", [128, 128], BF16)
    p.dma('pool', idb[:], ident)
    mhalf = p.sb("mhalf", [128, 8], F32)
    p.memset('dve', mhalf[:], -0.5)
    cs = p.sb("cs", [128, 16], F32)
    p.dma('sp', cs[:], cT)
    sg = p.sb("sg", [128, 16], F32)
    p.actv(sg[:], cs[:], AF.Sigmoid)
    p.tt('dve', cs[:], cs[:], sg[:], ALU.mult)
    modt = p.sb("modt", [128, 8192], F32)
    nrmb = p.sb("nrmb", [128, D], F32)
    p.dma('sp', nrmb[:], nrm.partition_broadcast(128))
    pa = [p.ps("pa%d" % i, [128, 512], F32) for i in range(4)]
    pb = [p.ps("pb%d" % i, [128, 512], F32) for i in range(2)]
    pc = p.ps("pc", [128, 1024], BF16)
    pd = p.ps("pd", [128, 512], F32)
    with ExitStack() as es2:
        crep = es2.enter_context(nc.sbuf_tensor("crep", [128, 16, 128], F32))
        awb = [es2.enter_context(nc.sbuf_tensor("awb%d" % i, [128, 16, 512], F32)) for i in range(2)]
        adb = [es2.enter_context(nc.sbuf_tensor("adb%d" % i, [128, 512], F32)) for i in range(2)]
        nfb = es2.enter_context(nc.sbuf_tensor("nfb", [128, D], F32))
        p.cp('dve', crep[:], cs[:].unsqueeze(2).to_broadcast([128, 16, 128]))
        p.dma('sp', nfb[:], nffn.partition_broadcast(128))
        awv = ada_w.rearrange("(kc p) n -> p kc n", p=128)
        for cc in range(16):
            b = awb[cc % 2]
            p.dma('sp', b[:], awv[:, :, cc * 512:(cc + 1) * 512])
            p.dma('sp', adb[cc % 2][:], ada_b[:, cc * 512:(cc + 1) * 512].partition_broadcast(128))
            for kc in range(16):
                p.mm(pb[cc % 2][:], crep[:, kc, :], b[:, kc, :], start=(kc == 0), stop=(kc == 15))
            p.tt('dve', modt[:, cc * 512:(cc + 1) * 512], pb[cc % 2][:], adb[cc % 2][:], ALU.add)
        p.stt('dve', modt[:, 2 * D:3 * D], modt[:, 2 * D:3 * D], 1.0, nfb[:], ALU.add, ALU.mult)
    p.barrier()
    g1 = modt[:, 0:D]
    sh2 = modt[:, D:2 * D]
    A2 = modt[:, 2 * D:3 * D]
    g2 = modt[:, 3 * D:4 * D]
    if last:
        nfinb = p.sb("nfinb", [128, D], F32)
        p.dma('sp', nfinb[:], nfin.partition_broadcast(128))
    skt = p.sb("skt", [128, 2, 128], F32)
    p.dma('sp', skt[:], skT)
    xt = p.sb("xt", [128, D], F32)
    at = p.sb("at", [128, 1024], F32)
    yt = p.sb("yt", [128, 1024], F32)
    zt = p.sb("zt", [128, 1024], F32)
    junk = p.sb("junk", [128, D], F32)
    cat = p.sb("cat", [128, D], BF16)
    catT = p.sb("catT", [128, 16, 128], BF16)
    h2T = p.sb("h2T", [128, 16, 128], BF16)
    ss = p.sb("ss", [128, 8], F32)
    wu = [p.sb("wu%d" % i, [128, 16, 512], BF16) for i in range(2)]
    wv = [p.sb("wv%d" % i, [128, 4, D], BF16) for i in range(2)]
    qT = p.sb("qT", [128, 16, 128], F32)
    sall = p.sb("sall", [128, 16, 128], F32)
    wk = p.sb("wk", [128, 256], F32)
    tv = p.sb("tv", [128, 2, 16], F32)
    cand = p.sb("cand", [128, 256], F32)
    best = p.sb("best", [128, 16], F32)
    eb = p.sb("eb", [128, 16], F32)
    thr = p.sb("thr", [128, 8], F32)
    nb = p.sb("nb", [128, 8], F32)
    zs = p.sb("zs", [128, 8], F32)
    nmx = p.sb("nmx", [128, 8], F32)
    Sb = [p.sb("Sb%d" % i, [128, 4, 128], F32) for i in range(2)]
    Eb = [p.sb("Eb%d" % i, [128, 4, 128], F32) for i in range(2)]
    Tb = [p.sb("Tb%d" % i, [128, 512], F32) for i in range(2)]
    Wacc = p.sb("Wacc", [128, 512], F32)
    Gt = p.sb("Gt", [128, 512], F32)
    GW = p.sb("GW", [128, 512], BF16)
    GT = [p.sb("GT%d" % i, [128, 4, 128], BF16) for i in range(2)]
    wctr = [0]

    def load_w(src_ap):
        b = wu[wctr[0] % 2]
        wctr[0] += 1
        p.dma('pool', b[:], src_ap)
        return b

    def transpose16(src_bf, dstT):
        for g in range(2):
            for j in range(8):
                kc = g * 8 + j
                p.tr(pc[:, j * 128:(j + 1) * 128], src_bf[:, kc * 128:(kc + 1) * 128], idb[:])
            p.cp('act' if g == 0 else 'dve', dstT[:, g * 8:(g + 1) * 8, :], pc[:].rearrange("p (a b) -> p a b", b=128))

    w_outv = w_out.rearrange("(kc p) n -> p kc n", p=128)
    wqv = wq.rearrange("(kc p) n -> p kc n", p=128)
    uTv = uT.rearrange("(kc p) n -> p kc n", p=128)
    vvv = vv.rearrange("(a p) d -> p a d", p=128)
    nt = ntok // 128
    for t in range(nt):
        r0 = t * 128
        p.dma('sp', at[:], attn[r0:r0 + 128, :])
        p.dma('sp', yt[:], yssm[r0:r0 + 128, :])
        p.dma('sp', zt[:], zz[r0:r0 + 128, :])
        p.dma('sp', xt[:], x[r0:r0 + 128, :])
        p.actv(junk[:, 0:1024], at[:], AF.Square, accum_out=ss[:, 0:1])
        p.actv(junk[:, 1024:2048], zt[:], AF.Sigmoid)
        p.tt('dve', yt[:], yt[:], zt[:], ALU.mult)
        p.tt('dve', yt[:], yt[:], junk[:, 1024:2048], ALU.mult)
        for g in range(2):
            p.actv(junk[:, g * 512:(g + 1) * 512], yt[:, g * 512:(g + 1) * 512], AF.Square, accum_out=ss[:, 1 + g:2 + g])
        p.ts('dve', ss[:, 0:1], ss[:, 0:1], 0.5, None, ALU.mult)
        rstd_op(p, ss[:, 0:3], mhalf[:, 0:3], 512.0)
        p.stt('dve', cat[:, 0:1024], at[:], ss[:, 0:1], nrmb[:, 0:1024], ALU.mult, ALU.mult)
        for g in range(2):
            p.stt('dve', cat[:, 1024 + g * 512:1024 + (g + 1) * 512], yt[:, g * 512:(g + 1) * 512], ss[:, 1 + g:2 + g],
                  nrmb[:, 1024 + g * 512:1024 + (g + 1) * 512], ALU.mult, ALU.mult)
        transpose16(cat, catT)
        for j in range(4):
            b = load_w(w_outv[:, :, j * 512:(j + 1) * 512])
            for kc in range(16):
                p.mm(pb[j % 2][:], catT[:, kc, :], b[:, kc, :], start=(kc == 0), stop=(kc == 15))
            p.tt('dve', junk[:, j * 512:(j + 1) * 512], pb[j % 2][:], g1[:, j * 512:(j + 1) * 512], ALU.mult)
        p.tt('dve', xt[:], xt[:], junk[:], ALU.add)
        p.actv(junk[:], xt[:], AF.Square, accum_out=ss[:, 3:4])
        rstd_op(p, ss[:, 3:4], mhalf[:, 0:1], float(D))
        p.stt('dve', junk[:], xt[:], ss[:, 3:4], A2, ALU.mult, ALU.mult)
        p.tt('dve', cat[:], junk[:], sh2, ALU.add)
        transpose16(cat, h2T)
        for j in range(4):
            b = load_w(wqv[:, :, j * 512:(j + 1) * 512])
            for c4 in range(4):
                for kc in range(16):
                    p.mm(pd[:, c4 * 128:(c4 + 1) * 128], b[:, kc, c4 * 128:(c4 + 1) * 128], h2T[:, kc, :],
                         start=(kc == 0), stop=(kc == 15))
            p.cp('act', qT[:, j * 4:(j + 1) * 4, :], pd[:].rearrange("p (a b) -> p a b", b=128))
        for j in range(4):
            for c4 in range(4):
                c16 = j * 4 + c4
                p.mm(pd[:, c4 * 128:(c4 + 1) * 128], qT[:, c16, :], skt[:, c16 % 2, :], start=True, stop=True)
            p.cp('act', sall[:, j * 4:(j + 1) * 4, :], pd[:].rearrange("p (a b) -> p a b", b=128))
        for h in range(8):
            for hf in range(2):
                s = sall[:, 2 * h + hf, :]
                p.op('dve', lambda e, s=s, hf=hf: e.max(tv[:, hf, 0:8], s), reads=[sall], writes=[tv])
                p.op('dve', lambda e, s=s, hf=hf: e.match_replace(wk[:, 0:128], tv[:, hf, 0:8], s, -1e30), reads=[sall, tv], writes=[wk])
                p.op('dve', lambda e, hf=hf: e.max(tv[:, hf, 8:16], wk[:, 0:128]), reads=[wk], writes=[tv])
            p.tt('dve', cand[:].rearrange("p (a b) -> p a b", b=16), tv[:, 0, :].unsqueeze(2).to_broadcast([128, 16, 16]),
                 tv[:, 1, :].unsqueeze(1).to_broadcast([128, 16, 16]), ALU.add)
            p.op('dve', lambda e: e.max(best[:, 0:8], cand[:]), reads=[cand], writes=[best])
            p.op('dve', lambda e: e.match_replace(wk[:], best[:, 0:8], cand[:], -1e30), reads=[cand, best], writes=[wk])
            p.op('dve', lambda e: e.max(best[:, 8:16], wk[:]), reads=[wk], writes=[best])
            p.cp('dve', thr[:, h:h + 1], best[:, 15:16])
            p.ts('dve', nmx[:, h:h + 1], best[:, 0:1], -1.0, None, ALU.mult)
            p.actv(eb[:], best[:], AF.Exp, bias=nmx[:, h:h + 1], scale=1.0, accum_out=zs[:, h:h + 1])
        p.actv(zs[:], zs[:], AF.Ln)
        p.tt('dve', nb[:], nmx[:], zs[:], ALU.subtract)
        for ec in range(nec):
            ub = load_w(uTv[:, :, ec * 512:(ec + 1) * 512])
            vb = wv[ec % 2]
            p.dma('pool', vb[:], vvv[:, ec * 4:(ec + 1) * 4, :])
            pbb = pb[ec % 2]
            for kc in range(16):
                p.mm(pbb[:], h2T[:, kc, :], ub[:, kc, :], start=(kc == 0), stop=(kc == 15))
            p.actv(Gt[:], pbb[:], AF.Gelu_apprx_tanh)
            for h in range(8):
                S_ = Sb[h % 2]
                E_ = Eb[h % 2]
                p.tt('dve', S_[:], sall[:, 2 * h, ec * 4:(ec + 1) * 4].unsqueeze(2).to_broadcast([128, 4, 128]),
                     sall[:, 2 * h + 1, :].unsqueeze(1).to_broadcast([128, 4, 128]), ALU.add)
                p.actv(E_[:], S_[:], AF.Exp, bias=nb[:, h:h + 1], scale=1.0)
                Sf = S_[:].rearrange("p a b -> p (a b)")
                Ef = E_[:].rearrange("p a b -> p (a b)")
                if h == 0:
                    p.stt('dve', Wacc[:], Sf, thr[:, h:h + 1], Ef, ALU.is_ge, ALU.mult)
                else:
                    T_ = Tb[h % 2]
                    p.stt('dve', T_[:], Sf, thr[:, h:h + 1], Ef, ALU.is_ge, ALU.mult)
                    p.tt('pool', Wacc[:], Wacc[:], T_[:], ALU.add)
            p.tt('dve', GW[:], Gt[:], Wacc[:], ALU.mult)
            half = (ec % 2) * 512
            for a in range(4):
                p.tr(pc[:, half + a * 128:half + (a + 1) * 128], GW[:, a * 128:(a + 1) * 128], idb[:])
            gt_ = GT[ec % 2]
            p.cp('act', gt_[:], pc[:, half:half + 512].rearrange("p (a b) -> p a b", b=128))
            for a in range(4):
                for n4 in range(4):
                    p.mm(pa[n4][:], gt_[:, a, :], vb[:, a, n4 * 512:(n4 + 1) * 512],
                         start=(ec == 0 and a == 0), stop=(ec == nec - 1 and a == 3))
        for n4 in range(4):
            p.tt('dve', junk[:, n4 * 512:(n4 + 1) * 512], pa[n4][:], g2[:, n4 * 512:(n4 + 1) * 512], ALU.mult)
        p.tt('dve', xt[:], xt[:], junk[:], ALU.add)
        if last:
            p.actv(junk[:], xt[:], AF.Square, accum_out=ss[:, 4:5])
            rstd_op(p, ss[:, 4:5], mhalf[:, 0:1], float(D))
            p.stt('dve', xt[:], xt[:], ss[:, 4:5], nfinb[:], ALU.mult, ALU.mult)
        p.dma('sp', out[r0:r0 + 128, :], xt[:])
    p.emit()
    return nc


def host_inputs_L3(inp, l, xs, attn, yssm, z, core):
    b = core // 4
    c = inp['c'][b]
    return {
        "x": xs, "attn": attn, "yssm": yssm, "z": z,
        "cT": np.ascontiguousarray(c.reshape(16, 128).T),
    }


_CACHE = {}


def _prog(name, fn):
    if name not in _CACHE:
        _CACHE[name] = fn()
    return _CACHE[name]


def kernel(x, c, ada_w, ada_b, norm_mix, norm_ffn, w_in, cmp_pe, cmp_w1, cmp_w2, rel_bias,
           attn_out_norm, conv_w, conv_b, dt_bias, a_log, d_skip, ssm_norm, w_out,
           peer_wq, peer_subkeys, peer_u, peer_v, norm_final):
    f = np.float32
    A = lambda a: np.ascontiguousarray(np.asarray(a, dtype=f))
    x = A(x); c = A(c)
    B, S, Dm = x.shape
    ident = np.eye(128, dtype=f)
    xcur = x.reshape(B * S, Dm)
    cTs = [np.ascontiguousarray(c[b].reshape(16, 128).T) for b in range(B)]
    depth = np.asarray(ada_w).shape[0]
    for l in range(depth):
        adw = np.asarray(ada_w[l], dtype=f)
        adb = np.asarray(ada_b[l], dtype=f)
        nc1 = _prog("L1", lambda: build_L1(4096))
        adw1 = np.ascontiguousarray(adw[:, :4096])
        w_in_l = A(w_in[l])
        maps = []
        for core in range(8):
            maps.append({"x": xcur[core * 4096:(core + 1) * 4096], "cT": cTs[core // 4], "ada_w": adw1,
                         "ada_b": adb[None, :4096].copy(), "nw": A(norm_mix[l])[None], "w_in": w_in_l, "ident": ident})
        res = run(nc1, maps)
        proj = np.concatenate([r["proj"] for r in res.results], 0).reshape(B, S, 4416)
        del maps, res
        q = proj[:, :, 0:1024]
        kv = proj[:, :, 1024:1792]
        gl = proj[:, :, 1792:1840]
        z = np.ascontiguousarray(proj[:, :, 1840:2864]).reshape(B * S, 1024)
        xbc = proj[:, :, 2864:4400]
        dtr = proj[:, :, 4400:4416]
        nc2a = _prog("L2a", lambda: build_L2a(S))
        maps = []
        for core in range(8):
            b, r = core // 4, core % 4
            g = r // 2
            maps.append(host_L2a(xbc[b][:, r * 256:(r + 1) * 256], xbc[b][:, 1024 + g * 128:1024 + (g + 1) * 128],
                                 xbc[b][:, 1280 + g * 128:1280 + (g + 1) * 128], dtr[b][:, 4 * r:4 * r + 4],
                                 A(conv_w[l]), A(conv_b[l]), A(dt_bias[l]), A(a_log[l]), A(d_skip[l]), r))
        res = run(nc2a, maps)
        yssm = np.empty((B, S, 1024), f)
        for core in range(8):
            b, r = core // 4, core % 4
            yssm[b, :, r * 256:(r + 1) * 256] = res.results[core]["y"]
        yssm = yssm.reshape(B * S, 1024)
        del maps, res
        nc2b = _prog("L2b", lambda: build_L2b(S))
        maps = []
        for core in range(8):
            b, r = core // 4, core % 4
            maps.append(host_L2b(q[b], kv[b], gl[b], A(cmp_pe[l]), A(cmp_w1[l]), A(cmp_w2[l]), A(rel_bias), r // 2, r % 2))
        res = run(nc2b, maps)
        attn = np.empty((B, S // 256, 2, 128, 2, 512), f)
        for core in range(8):
            b, r = core // 4, core % 4
            attn[b, :, r % 2, :, r // 2, :] = res.results[core]["o"].reshape(S // 256, 128, 512)
        attn = attn.reshape(B * S, 1024)
        del maps, res, proj
        last = (l == depth - 1)
        nc3 = _prog("L3_%d" % int(last), lambda: build_L3(4096, last=last))
        adw3 = np.ascontiguousarray(adw[:, 4096:])
        nrm = np.concatenate([A(attn_out_norm[l]), A(ssm_norm[l])])[None]
        skT = np.ascontiguousarray(A(peer_subkeys[l]).transpose(2, 0, 1))
        uT = np.ascontiguousarray(A(peer_u[l]).T)
        vv = A(peer_v[l])
        wo = A(w_out[l]); wq_ = A(peer_wq[l])
        maps = []
        for core in range(8):
            sl = slice(core * 4096, (core + 1) * 4096)
            maps.append({"x": xcur[sl], "attn": attn[sl], "yssm": yssm[sl], "z": z[sl], "cT": cTs[core // 4],
                         "ada_w": adw3, "ada_b": adb[None, 4096:].copy(), "nrm": nrm, "nffn": A(norm_ffn[l])[None],
                         "nfin": A(norm_final)[None], "w_out": wo, "wq": wq_, "skT": skT, "uT": uT, "v": vv, "ident": ident})
        res = run(nc3, maps)
        xcur = np.concatenate([r["out"] for r in res.results], 0)
        del maps, res, uT, vv
    return xcur.reshape(B, S, Dm).astype(np.float32)
```
